# Optimizing a Trainium2 kernel written in Bass

```python
import jax
import jax.numpy as jnp
from jax import lax
import numpy as np

D_MODEL = 2048
BATCH = 4
SEQ = 4096
DEPTH = 2

N_MIXERS = 4
GROUP_WIDTH = D_MODEL // N_MIXERS
MIX_WIDTH = N_MIXERS * GROUP_WIDTH
HEAD_DIM = 128
N_HEADS = GROUP_WIDTH // HEAD_DIM
LRU_BLOCKS = N_HEADS
LRU_BLOCK_WIDTH = GROUP_WIDTH // LRU_BLOCKS
LRU_CONV_WIDTH = 4
LRU_C = 8.0
MLSTM_CHUNK = 64
DILATED_PATTERNS = ((128, 1), (512, 4), (2048, 16))
DIL_BLOCK = 128
SB_BLOCK = 128
D_FF = ((8 * D_MODEL // 3 + 127) // 128) * 128
RMS_EPS = 1e-6
NEG_BIG = -1e30

IN_SIZES = (
    GROUP_WIDTH, GROUP_WIDTH,
    GROUP_WIDTH, GROUP_WIDTH, GROUP_WIDTH, GROUP_WIDTH, N_HEADS, N_HEADS,
    GROUP_WIDTH, GROUP_WIDTH, GROUP_WIDTH,
    GROUP_WIDTH, GROUP_WIDTH, GROUP_WIDTH,
)
D_IN = sum(IN_SIZES)

kernel_name = 'hybrid_parallel_groups_rglru_mlstm_dilated_stickbreaking'


def _split_points():
    pts, acc = [], 0
    for s in IN_SIZES[:-1]:
        acc += s
        pts.append(acc)
    return pts


def alibi_slopes():
    return jnp.asarray(2.0 ** (-8.0 * np.arange(1, N_HEADS + 1) / N_HEADS), dtype=jnp.float32)


def rmsnorm(x, gain):
    xf = x.astype(jnp.float32)
    y = xf * lax.rsqrt(jnp.mean(xf * xf, axis=-1, keepdims=True) + RMS_EPS)
    return (y * gain.astype(jnp.float32)).astype(x.dtype)


def swiglu(x, w_gate, w_up, w_down):
    return (jax.nn.silu(x @ w_gate) * (x @ w_up)) @ w_down


def rglru_mixer(xr, gate, conv_w, conv_b, w_a, b_a, w_x, b_x, lam):
    bsz, seq, width = xr.shape
    xc = lax.conv_general_dilated(xr, conv_w[:, None, :], window_strides=(1,),
                                  padding=[(LRU_CONV_WIDTH - 1, 0)],
                                  dimension_numbers=('NWC', 'WIO', 'NWC'),
                                  feature_group_count=width) + conv_b
    xg = xc.reshape(bsz, seq, LRU_BLOCKS, LRU_BLOCK_WIDTH)
    r = jax.nn.sigmoid(jnp.einsum('bsnc,ncd->bsnd', xg, w_a).reshape(bsz, seq, width) + b_a)
    i = jax.nn.sigmoid(jnp.einsum('bsnc,ncd->bsnd', xg, w_x).reshape(bsz, seq, width) + b_x)
    log_a = -LRU_C * jax.nn.softplus(-lam.astype(jnp.float32)) * r.astype(jnp.float32)
    a = jnp.exp(log_a)
    u = jnp.sqrt(-jnp.expm1(2.0 * log_a)) * (i * xc).astype(jnp.float32)

    def combine(lhs, rhs):
        a1, b1 = lhs
        a2, b2 = rhs
        return a1 * a2, a2 * b1 + b2

    _, h = lax.associative_scan(combine, (a, u), axis=1)
    return h.astype(xr.dtype) * jax.nn.gelu(gate)


def mlstm_mixer(q, k, v, o_pre, i_pre, f_pre, ig_bias, fg_bias, head_gain):
    bsz, seq, _ = q.shape
    nh, dh, ln = N_HEADS, HEAD_DIM, MLSTM_CHUNK
    nc = seq // ln

    def to_chunks(t):
        return t.astype(jnp.float32).reshape(bsz, nc, ln, nh, dh).transpose(1, 0, 3, 2, 4)

    def gate_chunks(t):
        return t.astype(jnp.float32).reshape(bsz, nc, ln, nh).transpose(1, 0, 3, 2)

    qc = to_chunks(q)
    kc = to_chunks(k) * (dh ** -0.5)
    vc = to_chunks(v)
    igc = gate_chunks(i_pre + ig_bias)
    lfc = jax.nn.log_sigmoid(gate_chunks(f_pre + fg_bias))
    causal = jnp.tril(jnp.ones((ln, ln), dtype=bool))

    def step(carry, inp):
        c_mat, n_vec, m_run = carry
        qt, kt, vt, ig, lf = inp
        b = jnp.cumsum(lf, axis=-1)
        log_d = jnp.where(causal, b[..., :, None] - b[..., None, :] + ig[..., None, :], -jnp.inf)
        inter = b + m_run[..., None]
        m_t = jnp.maximum(inter, log_d.max(axis=-1))
        s = jnp.einsum('bhtd,bhsd->bhts', qt, kt) * jnp.exp(log_d - m_t[..., None])
        w_inter = jnp.exp(inter - m_t)
        num = jnp.einsum('bhts,bhsd->bhtd', s, vt) + w_inter[..., None] * jnp.einsum('bhtk,bhkv->bhtv', qt, c_mat)
        den = s.sum(axis=-1) + w_inter * jnp.einsum('bhtk,bhk->bht', qt, n_vec)
        h = num / jnp.maximum(jnp.abs(den), jnp.exp(-m_t))[..., None]
        b_last = b[..., -1]
        log_w = b_last[..., None] - b + ig
        m_next = jnp.maximum(b_last + m_run, log_w.max(axis=-1))
        w = jnp.exp(log_w - m_next[..., None])
        decay = jnp.exp(b_last + m_run - m_next)
        c_mat = decay[..., None, None] * c_mat + jnp.einsum('bhs,bhsk,bhsv->bhkv', w, kt, vt)
        n_vec = decay[..., None] * n_vec + jnp.einsum('bhs,bhsk->bhk', w, kt)
        return (c_mat, n_vec, m_next), h

    init = (jnp.zeros((bsz, nh, dh, dh), jnp.float32),
            jnp.zeros((bsz, nh, dh), jnp.float32),
            jnp.zeros((bsz, nh), jnp.float32))
    _, hs = lax.scan(step, init, (qc, kc, vc, igc, lfc))
    h = hs.transpose(1, 0, 3, 2, 4).reshape(bsz, seq, nh, dh)
    h = h * lax.rsqrt(jnp.mean(h * h, axis=-1, keepdims=True) + RMS_EPS) * head_gain.astype(jnp.float32).reshape(nh, dh)
    h = h.reshape(bsz, seq, nh * dh) * jax.nn.sigmoid(o_pre.astype(jnp.float32))
    return h.astype(q.dtype)


def dilated_branch(q, k, v, window, dilation, slopes):
    bsz, seq, nh, dh = q.shape
    span = window // dilation
    sub_len = seq // dilation
    nb = -(-sub_len // DIL_BLOCK)
    pad_len = nb * DIL_BLOCK

    def to_sub(t):
        t = t.reshape(bsz, sub_len, dilation, nh, dh).transpose(0, 2, 3, 1, 4)
        t = jnp.pad(t, ((0, 0), (0, 0), (0, 0), (0, pad_len - sub_len), (0, 0)))
        return t.reshape(bsz, dilation, nh, nb, DIL_BLOCK, dh)

    def with_prev(t):
        prev = jnp.pad(t, ((0, 0), (0, 0), (0, 0), (1, 0), (0, 0), (0, 0)))[:, :, :, :-1]
        return jnp.concatenate([prev, t], axis=4)

    qs = to_sub(q)
    ks = with_prev(to_sub(k))
    vs = with_prev(to_sub(v)).astype(jnp.float32)
    scores = jnp.einsum('brhnqd,brhnkd->brhnqk', qs, ks).astype(jnp.float32) * (dh ** -0.5)
    q_pos = jnp.arange(DIL_BLOCK)[:, None] + DIL_BLOCK
    k_pos = jnp.arange(2 * DIL_BLOCK)[None, :]
    dist = q_pos - k_pos
    k_abs = (jnp.arange(nb) * DIL_BLOCK - DIL_BLOCK)[:, None, None] + k_pos[None]
    valid = (dist >= 0) & (dist <= span) & (k_abs >= 0)
    bias = -slopes[:, None, None, None] * (dilation * dist).astype(jnp.float32)
    scores = jnp.where(valid, scores + bias, NEG_BIG)
    m = scores.max(axis=-1)
    p = jnp.exp(scores - m[..., None])
    den = p.sum(axis=-1)
    num = jnp.einsum('brhnqk,brhnkd->brhnqd', p, vs)

    def from_sub(t):
        t = t.reshape((bsz, dilation, nh, pad_len) + t.shape[5:])[:, :, :, :sub_len]
        t = jnp.moveaxis(t, 3, 1)
        return t.reshape((bsz, seq, nh) + t.shape[4:])

    return from_sub(num), from_sub(den), from_sub(m)


def dilated_mixer(q, k, v, q_gain, k_gain):
    bsz, seq, _ = q.shape
    shp = (bsz, seq, N_HEADS, HEAD_DIM)
    qh = rmsnorm(q.reshape(shp), q_gain)
    kh = rmsnorm(k.reshape(shp), k_gain)
    vh = v.reshape(shp)
    slopes = alibi_slopes()
    outs = [dilated_branch(qh, kh, vh, w, d, slopes) for (w, d) in DILATED_PATTERNS]
    nums = jnp.stack([o[0] for o in outs])
    dens = jnp.stack([o[1] for o in outs])
    ms = jnp.stack([o[2] for o in outs])
    wts = jnp.exp(ms - ms.max(axis=0, keepdims=True))
    out = (wts[..., None] * nums).sum(axis=0) / (wts * dens).sum(axis=0)[..., None]
    return out.reshape(bsz, seq, GROUP_WIDTH).astype(q.dtype)


def stick_breaking_mixer(q, k, v):
    bsz, seq, _ = q.shape

    def heads(t):
        return t.reshape(bsz, seq, N_HEADS, HEAD_DIM).transpose(0, 2, 1, 3)

    qh, kh = heads(q), heads(k)
    vh = heads(v).astype(jnp.float32)
    key_pos = jnp.arange(seq)

    def block(n):
        start = n * SB_BLOCK
        qb = lax.dynamic_slice_in_dim(qh, start, SB_BLOCK, axis=2)
        z = jnp.einsum('bhqd,bhkd->bhqk', qb, kh).astype(jnp.float32) * (HEAD_DIM ** -0.5)
        q_pos = start + jnp.arange(SB_BLOCK)
        mask = key_pos[None, :] < q_pos[:, None]
        log_keep = jnp.where(mask, jax.nn.log_sigmoid(-z), 0.0)
        log_keep_after = lax.cumsum(log_keep, axis=3, reverse=True) - log_keep
        weight = jnp.where(mask, jnp.exp(jax.nn.log_sigmoid(z) + log_keep_after), 0.0)
        return jnp.einsum('bhqk,bhkd->bhqd', weight, vh)

    out = lax.map(block, jnp.arange(seq // SB_BLOCK))
    return out.transpose(1, 0, 3, 2, 4).reshape(bsz, seq, GROUP_WIDTH).astype(q.dtype)


def setup_inputs(seed: int = 0) -> dict:
    key = jax.random.key(seed)
    ks = jax.random.split(key, 24)

    def nrm(k, shape, scale):
        return jax.random.normal(k, shape, jnp.float32) * scale

    def gain(k, shape):
        return 1.0 + 0.02 * jax.random.normal(k, shape, jnp.float32)

    nl = DEPTH
    u = jax.random.uniform(ks[13], (nl, GROUP_WIDTH), jnp.float32, 0.9, 0.999)
    a = u ** (1.0 / LRU_C)
    lru_lambda = jnp.log(a) - jnp.log1p(-a)
    fg_bias = jnp.linspace(3.0, 6.0, N_HEADS, dtype=jnp.float32)[None, :] + nrm(ks[15], (nl, N_HEADS), 0.02)
    return {
        'x': nrm(ks[0], (BATCH, SEQ, D_MODEL), 1.0),
        'ffn1_norm': gain(ks[1], (nl, D_MODEL)),
        'ffn1_w_gate': nrm(ks[2], (nl, D_MODEL, D_FF), D_MODEL ** -0.5),
        'ffn1_w_up': nrm(ks[3], (nl, D_MODEL, D_FF), D_MODEL ** -0.5),
        'ffn1_w_down': nrm(ks[4], (nl, D_FF, D_MODEL), D_FF ** -0.5),
        'mix_norm': gain(ks[5], (nl, D_MODEL)),
        'w_in': nrm(ks[6], (nl, D_MODEL, D_IN), D_MODEL ** -0.5),
        'lru_conv_w': nrm(ks[7], (nl, LRU_CONV_WIDTH, GROUP_WIDTH), LRU_CONV_WIDTH ** -0.5),
        'lru_conv_b': nrm(ks[8], (nl, GROUP_WIDTH), 0.02),
        'lru_w_a': nrm(ks[9], (nl, LRU_BLOCKS, LRU_BLOCK_WIDTH, LRU_BLOCK_WIDTH), LRU_BLOCK_WIDTH ** -0.5),
        'lru_b_a': nrm(ks[10], (nl, GROUP_WIDTH), 0.02),
        'lru_w_x': nrm(ks[11], (nl, LRU_BLOCKS, LRU_BLOCK_WIDTH, LRU_BLOCK_WIDTH), LRU_BLOCK_WIDTH ** -0.5),
        'lru_b_x': nrm(ks[12], (nl, GROUP_WIDTH), 0.02),
        'lru_lambda': lru_lambda,
        'mlstm_ig_bias': nrm(ks[14], (nl, N_HEADS), 0.1),
        'mlstm_fg_bias': fg_bias,
        'attn_q_gain': gain(ks[16], (nl, HEAD_DIM)),
        'attn_k_gain': gain(ks[17], (nl, HEAD_DIM)),
        'group_out_gain': gain(ks[18], (nl, MIX_WIDTH)),
        'w_out': nrm(ks[19], (nl, MIX_WIDTH, D_MODEL), MIX_WIDTH ** -0.5),
        'ffn2_norm': gain(ks[20], (nl, D_MODEL)),
        'ffn2_w_gate': nrm(ks[21], (nl, D_MODEL, D_FF), D_MODEL ** -0.5),
        'ffn2_w_up': nrm(ks[22], (nl, D_MODEL, D_FF), D_MODEL ** -0.5),
        'ffn2_w_down': nrm(ks[23], (nl, D_FF, D_MODEL), D_FF ** -0.5),
    }


def reference(x, ffn1_norm, ffn1_w_gate, ffn1_w_up, ffn1_w_down, mix_norm, w_in,
              lru_conv_w, lru_conv_b, lru_w_a, lru_b_a, lru_w_x, lru_b_x, lru_lambda,
              mlstm_ig_bias, mlstm_fg_bias, attn_q_gain, attn_k_gain, group_out_gain, w_out,
              ffn2_norm, ffn2_w_gate, ffn2_w_up, ffn2_w_down):
    split_pts = _split_points()
    for l in range(DEPTH):
        x = x + 0.5 * swiglu(rmsnorm(x, ffn1_norm[l]), ffn1_w_gate[l], ffn1_w_up[l], ffn1_w_down[l])
        h = rmsnorm(x, mix_norm[l])
        (lx, lg, mq, mk, mv, mo, mi, mf, cq, ck, cv, sq, sk, sv) = jnp.split(h @ w_in[l], split_pts, axis=-1)
        g_a, g_b, g_c, g_d = jnp.split(group_out_gain[l], N_MIXERS)
        y_a = rmsnorm(rglru_mixer(lx, lg, lru_conv_w[l], lru_conv_b[l], lru_w_a[l], lru_b_a[l],
                                  lru_w_x[l], lru_b_x[l], lru_lambda[l]), g_a)
        y_b = mlstm_mixer(mq, mk, mv, mo, mi, mf, mlstm_ig_bias[l], mlstm_fg_bias[l], g_b)
        y_c = rmsnorm(dilated_mixer(cq, ck, cv, attn_q_gain[l], attn_k_gain[l]), g_c)
        y_d = rmsnorm(stick_breaking_mixer(sq, sk, sv), g_d)
        mixed = jnp.concatenate([y_a, y_b, y_c, y_d], axis=-1).astype(x.dtype)
        x = x + mixed @ w_out[l]
        x = x + 0.5 * swiglu(rmsnorm(x, ffn2_norm[l]), ffn2_w_gate[l], ffn2_w_up[l], ffn2_w_down[l])
    return x
```

```python
import numpy as np
import ml_dtypes
import concourse.bass as bass
import concourse.mybir as mybir
from concourse.bass_utils import run_bass_kernel_spmd


F32 = mybir.dt.float32
BF16 = mybir.dt.bfloat16
AF = mybir.ActivationFunctionType
ALU = mybir.AluOpType
AX = mybir.AxisListType


class Prog:
    ENGS = ("pe", "act", "dve", "pool", "sp")

    def __init__(self, nc, same_engine_sync=True):
        self.nc = nc
        self.ops = []
        self.same_engine_sync = same_engine_sync

    def op(self, eng, fn, reads=(), writes=(), dma_key=None):
        self.ops.append(dict(eng=eng, fn=fn, reads=tuple(reads), writes=tuple(writes),
                             dma_key=dma_key, deps=set(), signal=False))
        return len(self.ops) - 1

    def mm(self, out, lhsT, rhs, start, stop, reads, writes, **kw):
        return self.op("pe", lambda e: e.matmul(out, lhsT, rhs, start=start, stop=stop, **kw), reads, writes)

    def act(self, out, in_, func, reads, writes, **kw):
        return self.op("act", lambda e: e.activation(out, in_, func, **kw), reads, writes)

    def dma(self, eng, out, in_, reads, writes, key):
        return self.op(eng, lambda e: e.dma_start(out=out, in_=in_), reads, writes, dma_key=key)

    def analyze(self):
        last_w = {}
        readers = {}
        for i, o in enumerate(self.ops):
            deps = o["deps"]
            for r in o["reads"]:
                if r in last_w:
                    deps.add(last_w[r])
            for w in o["writes"]:
                if w in last_w:
                    deps.add(last_w[w])
                for rd in readers.get(w, ()):
                    deps.add(rd)
            deps.discard(i)
            for r in o["reads"]:
                readers.setdefault(r, []).append(i)
            for w in o["writes"]:
                last_w[w] = i
                readers[w] = []
        for i, o in enumerate(self.ops):
            keep = set()
            for j in o["deps"]:
                pj = self.ops[j]
                if pj["dma_key"] is None and pj["eng"] == o["eng"]:
                    if o["dma_key"] is None and (o["eng"] == "pe" or not self.same_engine_sync):
                        continue
                keep.add(j)
            o["deps"] = keep
            for j in keep:
                self.ops[j]["signal"] = True

    def emit(self, final_wait_keys=None):
        nc = self.nc
        self.analyze()
        handles = {"pe": nc.tensor, "act": nc.scalar, "dve": nc.vector, "pool": nc.gpsimd, "sp": nc.sync}
        esem = {e: nc.alloc_semaphore("s_" + e) for e in self.ENGS}
        dsem = {}
        for o in self.ops:
            if o["dma_key"] is not None and o["dma_key"] not in dsem:
                dsem[o["dma_key"]] = nc.alloc_semaphore("d_%d" % len(dsem))
        ecount = {e: 0 for e in self.ENGS}
        dcount = {k: 0 for k in dsem}
        for o in self.ops:
            if o["dma_key"] is not None:
                dcount[o["dma_key"]] += 16
                o["sig"] = (dsem[o["dma_key"]], dcount[o["dma_key"]])
            elif o["signal"]:
                ecount[o["eng"]] += 1
                o["sig"] = (esem[o["eng"]], ecount[o["eng"]])
            else:
                o["sig"] = None
        self.n_sems = len(dsem) + len(esem)
        dma_issuer = {}
        for o in self.ops:
            if o["dma_key"] is not None:
                dma_issuer.setdefault(o["eng"], set()).add(o["dma_key"])
        waited = {e: {} for e in self.ENGS}
        for e in self.ENGS:
            h = handles[e]
            wd = waited[e]
            for o in self.ops:
                if o["eng"] != e:
                    continue
                need = {}
                for j in o["deps"]:
                    s, v = self.ops[j]["sig"]
                    sid = id(s)
                    if need.get(sid, (None, 0))[1] < v:
                        need[sid] = (s, v)
                for sid, (s, v) in need.items():
                    if wd.get(sid, 0) >= v:
                        continue
                    wd[sid] = v
                    h.wait_ge(s, v)
                ins = o["fn"](h)
                if o["dma_key"] is not None:
                    ins.then_inc(o["sig"][0], 16)
                elif o["sig"] is not None:
                    ins.then_inc(o["sig"][0], 1)
            for k in dma_issuer.get(e, ()):
                if dcount[k] > 0 and wd.get(id(dsem[k]), 0) < dcount[k]:
                    h.wait_ge(dsem[k], dcount[k])

D = 2048
KC = 16
DFF = 5504
NF = 43
TT = 512
NW = 12
EPS = 1e-6
OFF = dict(lx=0, lg=512, mq=1024, mk=1536, mv=2048, mo=2560, mi=3072, mf=3076,
           cq=3080, ck=3592, cv=4104, sq=4616, sk=5128, sv=5640)
FM_NAMES = ["lx", "lg", "mq", "mk", "mo", "cq", "ck", "sq", "sk"]
TM_NAMES = ["mk", "mv", "cv", "sv"]
NFM = 36


def bf16(a):
    return a.astype(ml_dtypes.bfloat16)


def tile_fm(W, c0):
    return W[:, c0:c0 + 128].reshape(KC, 128, 128).transpose(1, 0, 2).reshape(128, 2048)


def tiles_ffn(wg, wu, wd):
    out = []
    for f in range(NF):
        out.append(tile_fm(wg, f * 128))
        out.append(tile_fm(wu, f * 128))
    wd_r = wd.reshape(NF, 128, D)
    for dc in range(KC):
        for f0 in (0, 16, 32):
            nf = min(16, NF - f0)
            t = np.zeros((128, 16, 128), np.float32)
            t[:, :nf, :] = wd_r[f0:f0 + nf, :, dc * 128:(dc + 1) * 128].transpose(1, 0, 2)
            out.append(t.reshape(128, 2048))
    return out


def tiles_win(w_in):
    out = []
    for nm in FM_NAMES:
        for c in range(4):
            out.append(tile_fm(w_in, OFF[nm] + c * 128))
    g = w_in[:, OFF["mi"]:OFF["mi"] + 8].reshape(KC, 128, 8).transpose(1, 0, 2).reshape(128, 128)
    t = np.zeros((128, 2048), np.float32)
    t[:, :128] = g
    out.append(t)
    for nm in TM_NAMES:
        blk = w_in[:, OFF[nm]:OFF[nm] + 512].reshape(KC, 128, 512)
        for pc in range(4):
            out.append(blk[pc * 4:(pc + 1) * 4].transpose(1, 0, 2).reshape(128, 2048))
    return out


def tiles_wout(w_out):
    return [tile_fm(w_out, dc * 128) for dc in range(KC)]


def gain_fm(g):
    return np.ascontiguousarray(g.reshape(-1, 128).T)


def build_T(has_wout, n_ffn, has_win, ntok=2048, debug_nf=None):
    nf_ = NF if debug_nf is None else debug_nf
    nc = bass.Bass("TRN2", target_bir_lowering=False)
    ntt = ntok // TT
    ntiles = (16 if has_wout else 0) + n_ffn * 134 + (53 if has_win else 0)
    ngain = 16 * (n_ffn + int(has_wout) + int(has_win))
    xT = nc.dram_tensor("xT", [128, KC, ntok], F32, kind="ExternalInput").ap()
    wt = nc.dram_tensor("wt", [ntiles, 128, 2048], BF16, kind="ExternalInput").ap()
    gains = nc.dram_tensor("gains", [128, ngain], F32, kind="ExternalInput").ap()
    oT = nc.dram_tensor("oT", [128, KC, ntok], F32, kind="ExternalOutput").ap()
    if has_wout:
        Yd = nc.dram_tensor("Y", [128, KC, ntok], F32, kind="ExternalInput").ap()
    if has_win:
        pf = nc.dram_tensor("pf", [128, NFM, ntok], F32, kind="ExternalOutput").ap()
        pg = nc.dram_tensor("pg", [8, ntok], F32, kind="ExternalOutput").ap()
        pt = nc.dram_tensor("pt", [ntok, 2048], BF16, kind="ExternalOutput").ap()

    xs = nc.alloc_sbuf_tensor("xs", [128, KC, TT], F32)
    hT = nc.alloc_sbuf_tensor("hT", [128, KC, TT], BF16)
    actT = nc.alloc_sbuf_tensor("actT", [128, NF, TT], BF16)
    wr = nc.alloc_sbuf_tensor("wr", [128, NW, 2048], BF16)
    gs = nc.alloc_sbuf_tensor("gs", [128, ngain], F32)
    ones = nc.alloc_sbuf_tensor("ones", [128, 128], BF16)
    sg = [nc.alloc_sbuf_tensor("sg%d" % i, [128, TT], F32) for i in range(2)]
    rs = nc.alloc_sbuf_tensor("rs", [128, TT], F32)
    st32 = [nc.alloc_sbuf_tensor("st32_%d" % i, [128, TT], F32) for i in range(2)]
    st16 = [nc.alloc_sbuf_tensor("st16_%d" % i, [128, TT], BF16) for i in range(2)]
    if has_wout:
        ys = nc.alloc_sbuf_tensor("ys", [128, KC, TT], F32)
    banks = [nc.alloc_psum_tensor("bank%d" % i, [128, TT], F32) for i in range(8)]

    P = Prog(nc)
    st = dict(w=0, bank=0, sg=0, s32=0, s16=0, wtile=0)

    def next_bank():
        b = st["bank"] % 8
        st["bank"] += 1
        return b

    def load_w(tile_idx):
        s = st["w"] % NW
        st["w"] += 1
        P.dma("sp", wr[:, s, :], wt[tile_idx], [], [("w", s)], ("w", s))
        return s

    P.dma("pool", gs[:], gains[:, :], [], ["gs"], "gs")
    P.op("dve", lambda e: e.memset(ones[:], 1.0), [], ["ones"])

    def rmsnorm(gcol0, dim_scale):
        P.act(actT[:, 0:KC, :], xs[:, :, :], AF.Square,
              [("xs", k) for k in range(KC)], [("act", k) for k in range(KC)])
        b = next_bank()
        for k in range(KC):
            P.mm(banks[b][:], ones[:], actT[:, k, :], k == 0, k == KC - 1,
                 ["ones", ("act", k)], [("ps", b)])
        P.act(rs[:], banks[b][:], AF.Sqrt, [("ps", b)], ["rs"], scale=dim_scale, bias=EPS)
        P.op("dve", lambda e: e.reciprocal(rs[:], rs[:]), ["rs"], ["rs"])
        for k in range(KC):
            P.op("dve", lambda e, k=k: e.scalar_tensor_tensor(
                hT[:, k, :], xs[:, k, :], gs[:, gcol0 + k:gcol0 + k + 1], rs[:], ALU.mult, ALU.mult),
                [("xs", k), "gs", "rs"], [("h", k)])

    for tt in range(ntt):
        t0 = tt * TT
        wi = 0
        gi = 0
        P.dma("pool", xs[:], xT[:, :, t0:t0 + TT], [], [("xs", k) for k in range(KC)], "xs")
        if has_wout:
            P.dma("pool", ys[:], Yd[:, :, t0:t0 + TT], [], [("ys", k) for k in range(KC)], "ys")
            for g in range(4):
                if g == 1:
                    P.op("pool", lambda e: e.tensor_copy(hT[:, 4:8, :], ys[:, 4:8, :]),
                         [("ys", k) for k in range(4, 8)], [("h", k) for k in range(4, 8)])
                    continue
                ks = list(range(g * 4, g * 4 + 4))
                P.act(actT[:, g * 4:g * 4 + 4, :], ys[:, g * 4:g * 4 + 4, :], AF.Square,
                      [("ys", k) for k in ks], [("act", k) for k in ks])
                b = next_bank()
                for i, k in enumerate(ks):
                    P.mm(banks[b][:], ones[:], actT[:, k, :], i == 0, i == 3, ["ones", ("act", k)], [("ps", b)])
                rsg = sg[g % 2]
                P.act(rsg[:], banks[b][:], AF.Sqrt, [("ps", b)], [("sg", g % 2)], scale=1.0 / 512, bias=EPS)
                P.op("dve", lambda e, rsg=rsg: e.reciprocal(rsg[:], rsg[:]), [("sg", g % 2)], [("sg", g % 2)])
                for k in ks:
                    P.op("dve", lambda e, k=k, rsg=rsg, gi=gi: e.scalar_tensor_tensor(
                        hT[:, k, :], ys[:, k, :], gs[:, gi + k:gi + k + 1], rsg[:], ALU.mult, ALU.mult),
                        [("ys", k), "gs", ("sg", g % 2)], [("h", k)])
            gi += 16
            for dc in range(KC):
                s = load_w(wi); wi += 1
                b = next_bank()
                for cc in range(KC):
                    P.mm(banks[b][:], wr[:, s, cc * 128:(cc + 1) * 128], hT[:, cc, :], cc == 0, cc == KC - 1,
                         [("w", s), ("h", cc)], [("ps", b)])
                P.op("dve", lambda e, dc=dc, b=b: e.tensor_tensor(xs[:, dc, :], banks[b][:], xs[:, dc, :], ALU.add),
                     [("ps", b), ("xs", dc)], [("xs", dc)])
        for fi in range(n_ffn):
            rmsnorm(gi, 1.0 / D)
            gi += 16
            for f in range(NF):
                sgt = load_w(wi); wi += 1
                sut = load_w(wi); wi += 1
                if f >= nf_:
                    continue
                bg = next_bank()
                bu = next_bank()
                for k in range(KC):
                    P.mm(banks[bg][:], wr[:, sgt, k * 128:(k + 1) * 128], hT[:, k, :], k == 0, k == KC - 1,
                         [("w", sgt), ("h", k)], [("ps", bg)])
                for k in range(KC):
                    P.mm(banks[bu][:], wr[:, sut, k * 128:(k + 1) * 128], hT[:, k, :], k == 0, k == KC - 1,
                         [("w", sut), ("h", k)], [("ps", bu)])
                q = st["sg"] % 2
                st["sg"] += 1
                P.act(sg[q][:], banks[bg][:], AF.Silu, [("ps", bg)], [("sg", q)])
                P.op("dve", lambda e, q=q, bu=bu, f=f: e.tensor_tensor(actT[:, f, :], sg[q][:], banks[bu][:], ALU.mult),
                     [("sg", q), ("ps", bu)], [("act", f)])
            for dc in range(KC):
                b = next_bank()
                for pc, f0 in enumerate((0, 16, 32)):
                    s = load_w(wi); wi += 1
                    nfp = min(16, nf_ - f0)
                    for j in range(nfp):
                        f = f0 + j
                        P.mm(banks[b][:], wr[:, s, j * 128:(j + 1) * 128], actT[:, f, :], f == 0, f == nf_ - 1,
                             [("w", s), ("act", f)], [("ps", b)])
                P.op("dve", lambda e, dc=dc, b=b: e.scalar_tensor_tensor(
                    xs[:, dc, :], banks[b][:], 0.5, xs[:, dc, :], ALU.mult, ALU.add),
                    [("ps", b), ("xs", dc)], [("xs", dc)])
        P.dma("pool", oT[:, :, t0:t0 + TT], xs[:], [("xs", k) for k in range(KC)], [("oT", tt)], "xs_st")
        if has_win:
            rmsnorm(gi, 1.0 / D)
            gi += 16
            for c in range(NFM):
                s = load_w(wi); wi += 1
                b = next_bank()
                for k in range(KC):
                    P.mm(banks[b][:], wr[:, s, k * 128:(k + 1) * 128], hT[:, k, :], k == 0, k == KC - 1,
                         [("w", s), ("h", k)], [("ps", b)])
                q = st["s32"] % 2
                st["s32"] += 1
                if c % 2 == 0:
                    P.act(st32[q][:], banks[b][:], AF.Copy, [("ps", b)], [("s32", q)])
                else:
                    P.op("dve", lambda e, q=q, b=b: e.tensor_copy(st32[q][:], banks[b][:]), [("ps", b)], [("s32", q)])
                P.dma("pool", pf[:, c, t0:t0 + TT], st32[q][:], [("s32", q)], [("pf", c, tt)], ("s32", q))
            s = load_w(wi); wi += 1
            b = next_bank()
            for k in range(KC):
                P.mm(banks[b][0:8, :], wr[:, s, k * 8:(k + 1) * 8], hT[:, k, :], k == 0, k == KC - 1,
                     [("w", s), ("h", k)], [("ps", b)])
            q = st["s32"] % 2
            st["s32"] += 1
            P.op("dve", lambda e, q=q, b=b: e.tensor_copy(st32[q][0:8, :], banks[b][0:8, :]), [("ps", b)], [("s32", q)])
            P.dma("pool", pg[:, t0:t0 + TT], st32[q][0:8, :], [("s32", q)], [("pg", tt)], ("s32", q))
            for gidx in range(4):
                bs = [next_bank() for _ in range(4)]
                for pc in range(4):
                    s = load_w(wi); wi += 1
                    for tb in range(4):
                        for j in range(4):
                            k = pc * 4 + j
                            P.mm(banks[bs[tb]][:], hT[:, k, tb * 128:(tb + 1) * 128], wr[:, s, j * 512:(j + 1) * 512],
                                 k == 0, k == KC - 1, [("w", s), ("h", k)], [("ps", bs[tb])])
                for tb in range(4):
                    q = st["s16"] % 2
                    st["s16"] += 1
                    if tb % 2 == 0:
                        P.act(st16[q][:], banks[bs[tb]][:], AF.Copy, [("ps", bs[tb])], [("s16", q)])
                    else:
                        P.op("dve", lambda e, q=q, b=bs[tb]: e.tensor_copy(st16[q][:], banks[b][:]),
                             [("ps", bs[tb])], [("s16", q)])
                    P.dma("pool", pt[t0 + tb * 128:t0 + (tb + 1) * 128, gidx * 512:(gidx + 1) * 512], st16[q][:],
                          [("s16", q)], [("pt", gidx, tt, tb)], ("s16", q))
        assert wi == ntiles, (wi, ntiles)
    P.emit()
    return nc, ntiles, P

S = 4096
NB = 32
SCALE = 128 ** -0.5
NEG = -30000.0
EPS = 1e-6
LX, LG, MQ, MK, MO, CQ, CK, SQ, SK = range(9)
CB_ONES, CB_UTN, CB_ONESN, CB_ID, CB_M01, CB_NEGM = 0, 128, 256, 384, 512, 512 + 2048
NCB = 512 + 4096
CF_DIL, CF_MM, CF_ONE, CF_ID, CF_NEG1, CF_LT, CF_SEL = 0, 1536, 1664, 1792, 1920, 2048, 2176
NCF = 2176 + 128
NPRM = 24


def make_consts(hp):
    cb = np.zeros((128, NCB), np.float32)
    j = np.arange(128)[:, None]
    s = np.arange(128)[None, :]
    cb[:, CB_ONES:CB_ONES + 128] = 1.0
    cb[:, CB_UTN:CB_UTN + 128] = -(j >= s).astype(np.float32)
    cb[:, CB_ONESN:CB_ONESN + 128] = -1.0
    cb[:, CB_ID:CB_ID + 128] = np.eye(128, dtype=np.float32)
    t = np.arange(512)[None, :]
    for i in range(4):
        valid = (j + 128 * i) < t
        cb[:, CB_M01 + i * 512:CB_M01 + (i + 1) * 512] = valid
        cb[:, CB_NEGM + i * 512:CB_NEGM + (i + 1) * 512] = np.where(valid, 0.0, NEG)
    cf = np.zeros((128, NCF), np.float32)
    slopes = 2.0 ** (-8.0 * np.arange(1, 5) / 4)
    kk = np.arange(128)[:, None]
    qq = np.arange(128)[None, :]
    for p, d in enumerate((1, 4, 16)):
        for hl in range(2):
            sl = slopes[hp * 2 + hl]
            dist = qq + 128 - kk
            prev = np.where(kk >= qq, -sl * d * dist, NEG)
            dist = qq - kk
            cur = np.where(kk <= qq, -sl * d * dist, NEG)
            o = CF_DIL + ((p * 2 + hl) * 2) * 128
            cf[:, o:o + 128] = prev
            cf[:, o + 128:o + 256] = cur
    cf[:, CF_MM:CF_MM + 128] = np.where(kk <= qq, 0.0, NEG)
    cf[:, CF_ONE:CF_ONE + 128] = 1.0
    cf[:, CF_ID:CF_ID + 128] = np.eye(128)
    cf[:, CF_NEG1:CF_NEG1 + 128] = -1.0
    cf[127, CF_SEL:CF_SEL + 128] = 1.0
    cf[:, CF_LT:CF_LT + 128] = (kk <= qq)
    return cb.astype(ml_dtypes.bfloat16), cf


def build_B(do=("lru", "mlstm", "dil", "sb"), stage=99):
    nc = bass.Bass("TRN2", target_bir_lowering=False)
    pf = nc.dram_tensor("pf", [128, 18, S], F32, kind="ExternalInput").ap()
    pg = nc.dram_tensor("pg", [4, S], F32, kind="ExternalInput").ap()
    pt = nc.dram_tensor("pt", [S, 1024], BF16, kind="ExternalInput").ap()
    prm = nc.dram_tensor("prm", [128, NPRM], F32, kind="ExternalInput").ap()
    wax = nc.dram_tensor("wax", [128, 4, 128], F32, kind="ExternalInput").ap()
    cbd = nc.dram_tensor("cb", [128, NCB], BF16, kind="ExternalInput").ap()
    cfd = nc.dram_tensor("cf", [128, NCF], F32, kind="ExternalInput").ap()
    Y = nc.dram_tensor("Y", [128, 8, S], F32, kind="ExternalOutput").ap()

    Fb = [nc.alloc_sbuf_tensor("F%d" % i, [128, S], F32) for i in range(5)]
    Bb = [nc.alloc_sbuf_tensor("B%d" % i, [128, S], BF16) for i in range(4)]
    T0 = nc.alloc_sbuf_tensor("T0", [128, NB, 256], BF16)
    T1 = nc.alloc_sbuf_tensor("T1", [128, NB, 128], BF16)
    T2 = nc.alloc_sbuf_tensor("T2", [128, NB, 128], BF16)
    T3 = nc.alloc_sbuf_tensor("T3", [128, NB, 128], BF16)
    Sf = [nc.alloc_sbuf_tensor("Sf%d" % i, [128, 256], F32) for i in range(2)]
    w32 = [nc.alloc_sbuf_tensor("w32_%d" % i, [128, 512], F32) for i in range(4)]
    w16 = [nc.alloc_sbuf_tensor("w16_%d" % i, [128, 512], BF16) for i in range(6)]
    wW = [nc.alloc_sbuf_tensor("wW_%d" % i, [128, 512], BF16) for i in range(3)]
    cb = nc.alloc_sbuf_tensor("cbs", [128, NCB], BF16)
    cf = nc.alloc_sbuf_tensor("cfs", [128, NCF], F32)
    pr = nc.alloc_sbuf_tensor("prs", [128, NPRM], F32)
    pr2 = nc.alloc_sbuf_tensor("prs2", [128, 8], F32)
    wa32 = nc.alloc_sbuf_tensor("wa32", [128, 4, 128], F32)
    wa16 = nc.alloc_sbuf_tensor("wa16", [128, 4, 128], BF16)
    smalls_t = nc.alloc_sbuf_tensor("smalls", [128, 256], F32)
    G32 = nc.alloc_sbuf_tensor("G32", [32, 4, 128], F32)
    smalls = smalls_t[:]
    Sbf = Fb[4][:].bitcast(BF16).rearrange("p (c x) -> p c x", c=NB)
    banks = [nc.alloc_psum_tensor("bank%d" % i, [128, 512], F32) for i in range(8)]

    P = Prog(nc)
    st = dict(bank=0, w32=0, w16=0, wW=0)
    ones_b = cb[:, CB_ONES:CB_ONES + 128]

    def next_bank(lo=0, hi=8):
        b = lo + st["bank"] % (hi - lo)
        st["bank"] += 1
        return b

    def n32():
        i = st["w32"] % 4
        st["w32"] += 1
        return i

    def n16():
        i = st["w16"] % 6
        st["w16"] += 1
        return i

    P.dma("sp", cb[:], cbd[:, :], [], ["cb"], "cb")
    P.dma("sp", cf[:], cfd[:, :], [], ["cf"], "cf")
    P.dma("sp", pr[:], prm[:, :], [], ["pr"], "pr")
    P.dma("sp", wa32[:], wax[:, :, :], [], ["wa32"], "wa32")
    P.op("dve", lambda e: e.tensor_copy(wa16[:], wa32[:]), ["wa32"], ["wa16"])

    def F(i):
        return ("F", i)

    def Bk(i):
        return ("B", i)

    def store_y(idx, src_ap, src_keys, bufname):
        P.dma("sp", Y[:, idx, :], src_ap, src_keys, [("Y", idx)], ("yst", bufname))

    def load_tm(dst, w, c0, key):
        for q4 in range(8):
            P.dma("sp", dst[:, q4 * 4:(q4 + 1) * 4, 0:w],
                  pt[q4 * 512:(q4 + 1) * 512, c0:c0 + w].rearrange("(n p) c -> p n c", p=128), [], [key], (key, q4 % 2))

    def lru(j):
        c0 = j * 8
        lx, lg, xc, r_, i_ = Fb[0], Fb[1], Fb[2], Fb[3], Fb[4]
        P.dma("sp", lx[:], pf[:, LX * 2 + j, :], [], [F(0)], F(0))
        P.dma("sp", lg[:], pf[:, LG * 2 + j, :], [], [F(1)], F(1))
        P.act(pr2[:, 0:1], pr[:, c0 + 7:c0 + 8], AF.Exp, ["pr"], ["pr2"], scale=-1.0)
        P.act(pr2[:, 0:1], pr2[:, 0:1], AF.Ln, ["pr2"], ["pr2"], bias=1.0)
        P.op("dve", lambda e: e.tensor_scalar(pr2[:, 1:2], pr2[:, 0:1], -16.0, None, ALU.mult), ["pr2"], ["pr2"])
        P.op("dve", lambda e: e.tensor_scalar(pr2[:, 0:1], pr2[:, 0:1], -8.0, None, ALU.mult), ["pr2"], ["pr2"])
        P.op("dve", lambda e: e.tensor_scalar(xc[:], lx[:], pr[:, c0 + 3:c0 + 4], pr[:, c0 + 4:c0 + 5], ALU.mult, ALU.add),
             [F(0), "pr"], [F(2)])
        for tap in range(3):
            sh = 3 - tap
            P.op("dve", lambda e, tap=tap, sh=sh: e.scalar_tensor_tensor(
                xc[:, sh:], lx[:, :S - sh], pr[:, c0 + tap:c0 + tap + 1], xc[:, sh:], ALU.mult, ALU.add),
                [F(0), F(2), "pr"], [F(2)])
        xcb = Bb[0]
        P.op("pool", lambda e: e.tensor_copy(xcb[:], xc[:]), [F(2)], [Bk(0)])
        for tl in range(8):
            sl = slice(tl * 512, (tl + 1) * 512)
            ba = next_bank()
            P.mm(banks[ba][:], wa16[:, j * 2, :], xcb[:, sl], True, True, ["wa16", Bk(0)], [("ps", ba)])
            bx = next_bank()
            P.mm(banks[bx][:], wa16[:, j * 2 + 1, :], xcb[:, sl], True, True, ["wa16", Bk(0)], [("ps", bx)])
            P.act(r_[:, sl], banks[ba][:], AF.Sigmoid, [("ps", ba), "pr"], [F(3)], bias=pr[:, c0 + 5:c0 + 6])
            P.act(i_[:, sl], banks[bx][:], AF.Sigmoid, [("ps", bx), "pr"], [F(4)], bias=pr[:, c0 + 6:c0 + 7])
        a_ = Fb[0]
        P.act(a_[:], r_[:], AF.Exp, [F(3), "pr2"], [F(0)], scale=pr2[:, 0:1])
        P.act(r_[:], r_[:], AF.Exp, [F(3), "pr2"], [F(3)], scale=pr2[:, 1:2])
        P.act(r_[:], r_[:], AF.Sqrt, [F(3)], [F(3)], scale=-1.0, bias=1.0)
        P.op("dve", lambda e: e.tensor_tensor(i_[:], i_[:], xc[:], ALU.mult), [F(4), F(2)], [F(4)])
        P.op("dve", lambda e: e.tensor_tensor(i_[:], i_[:], r_[:], ALU.mult), [F(4), F(3)], [F(4)])
        P.op("dve", lambda e: e.tensor_tensor_scan(r_[:], a_[:], i_[:], 0.0, ALU.mult, ALU.add),
             [F(0), F(4)], [F(3)])
        g = Fb[2]
        P.act(g[:], lg[:], AF.Square, [F(1)], [F(2)])
        P.op("dve", lambda e: e.tensor_scalar(g[:], g[:], 0.044715, 1.0, ALU.mult, ALU.add), [F(2)], [F(2)])
        P.op("dve", lambda e: e.tensor_tensor(g[:], g[:], lg[:], ALU.mult), [F(2), F(1)], [F(2)])
        P.act(g[:], g[:], AF.Sigmoid, [F(2)], [F(2)], scale=1.5957691216057308)
        P.op("dve", lambda e: e.tensor_tensor(g[:], g[:], lg[:], ALU.mult), [F(2), F(1)], [F(2)])
        P.op("dve", lambda e: e.tensor_tensor(g[:], g[:], r_[:], ALU.mult), [F(2), F(3)], [F(2)])
        store_y(0 * 2 + j, g[:], [F(2)], "F2")

    def sb(hl):
        q32, k32 = Fb[0], Fb[1]
        qb, kb_ = Bb[0], Bb[1]
        vt = T1
        P.dma("sp", q32[:], pf[:, SQ * 2 + hl, :], [], [F(0)], F(0))
        P.dma("sp", k32[:], pf[:, SK * 2 + hl, :], [], [F(1)], F(1))
        c0 = (3 * 2 + hl) * 128
        load_tm(vt, 128, c0, "T1")
        P.op("dve", lambda e: e.tensor_scalar(qb[:], q32[:], SCALE, None, ALU.mult), [F(0)], [Bk(0)])
        P.op("pool", lambda e: e.tensor_copy(kb_[:], k32[:]), [F(1)], [Bk(1)])
        spsum = [w16[4], w16[5]]
        ost = Fb[2]
        for qg in range(8):
            qs = slice(qg * 512, (qg + 1) * 512)
            ob = 6 + qg % 2
            nkb = 4 * qg + 4
            for it in range(nkb):
                kb = nkb - 1 - it
                diag = kb - 4 * qg
                ks = slice(kb * 128, (kb + 1) * 128)
                zb = next_bank(0, 3)
                P.mm(banks[zb][:], kb_[:, ks], qb[:, qs], True, True, [Bk(0), Bk(1)], [("ps", zb)])
                e32 = n32()
                P.act(w32[e32][:], banks[zb][:], AF.Exp, [("ps", zb)], [("w32", e32)])
                sp = st["w16"] % 4
                st["w16"] += 1
                P.act(w16[sp][:], w32[e32][:], AF.Ln, [("w32", e32)], [("w16", sp)], bias=1.0)
                if diag >= 0:
                    P.op("dve", lambda e, sp=sp, diag=diag: e.tensor_tensor(
                        w16[sp][:], w16[sp][:], cb[:, CB_M01 + diag * 512:CB_M01 + (diag + 1) * 512], ALU.mult),
                        [("w16", sp), "cb"], [("w16", sp)])
                ab = 3 + next_bank(0, 3)
                last_is_id = diag >= 0
                P.mm(banks[ab][:], kb_[:, ks], qb[:, qs], True, False, [Bk(0), Bk(1)], [("ps", ab)])
                has_sum = it > 0
                P.mm(banks[ab][:], cb[:, CB_UTN:CB_UTN + 128], w16[sp][:], False, not (has_sum or last_is_id),
                     ["cb", ("w16", sp)], [("ps", ab)])
                if has_sum:
                    P.mm(banks[ab][:], cb[:, CB_ONESN:CB_ONESN + 128], spsum[it % 2][:], False, not last_is_id,
                         ["cb", ("sps", it % 2)], [("ps", ab)])
                if last_is_id:
                    P.mm(banks[ab][:], cb[:, CB_ID:CB_ID + 128], cb[:, CB_NEGM + diag * 512:CB_NEGM + (diag + 1) * 512],
                         False, True, ["cb"], [("ps", ab)])
                if it < nkb - 1:
                    if it == 0:
                        P.op("pool", lambda e, sp=sp: e.tensor_copy(spsum[1][:], w16[sp][:]),
                             [("w16", sp)], [("sps", 1)])
                    else:
                        P.op("pool", lambda e, sp=sp, it=it: e.tensor_tensor(
                            spsum[(it + 1) % 2][:], spsum[it % 2][:], w16[sp][:], ALU.add),
                            [("w16", sp), ("sps", it % 2)], [("sps", (it + 1) % 2)])
                wv = st["wW"] % 3
                st["wW"] += 1
                wb = wW[wv][:]
                P.act(wb, banks[ab][:], AF.Exp, [("ps", ab)], [("wW", wv)])
                P.mm(banks[ob][:], vt[:, kb, :], wb, it == 0, it == nkb - 1, ["T1", ("wW", wv)], [("ps", ob)])
            if qg % 2 == 0:
                P.act(ost[:, qs], banks[ob][:], AF.Copy, [("ps", ob), F(2)], [("F2q", qg)])
            else:
                P.op("dve", lambda e, ob=ob, qs=qs: e.tensor_copy(ost[:, qs], banks[ob][:]), [("ps", ob), F(2)], [("F2q", qg)])
        store_y(3 * 2 + hl, ost[:], [("F2q", g) for g in range(8)] + [F(2)], "F2")

    def dil(hl):
        q32, k32, Nacc, Dacc = Fb[0], Fb[1], Fb[2], Fb[3]
        qn, kn = Bb[0], Bb[1]
        vts = [T1, T2, T3]
        P.dma("sp", q32[:], pf[:, CQ * 2 + hl, :], [], [F(0)], F(0))
        P.dma("sp", k32[:], pf[:, CK * 2 + hl, :], [], [F(1)], F(1))
        c0 = (2 * 2 + hl) * 128
        vsrc = pt[:, c0:c0 + 128]
        load_tm(T1, 128, c0, "T1")
        for r in range(4):
            P.dma("sp", T2[:, r * 8:(r + 1) * 8, :], vsrc[r:S:4, :].rearrange("(n p) c -> p n c", p=128),
                  [], ["T2"], ("T2", r))
        for r in range(16):
            P.dma("sp", T3[:, r * 2:(r + 1) * 2, :], vsrc[r:S:16, :].rearrange("(n p) c -> p n c", p=128),
                  [], ["T3"], ("T3", r % 4))
        for (src, dst, gcol, sk, dk) in ((q32, qn, 16, F(0), Bk(0)), (k32, kn, 17, F(1), Bk(1))):
            for tl in range(8):
                sl = slice(tl * 512, (tl + 1) * 512)
                sq = n16() % 4
                P.act(w16[sq][:], src[:, sl], AF.Square, [sk], [("w16", sq)])
                b = next_bank()
                P.mm(banks[b][:], ones_b, w16[sq][:], True, True, ["cb", ("w16", sq)], [("ps", b)])
                r32 = n32()
                P.act(w32[r32][:], banks[b][:], AF.Sqrt, [("ps", b)], [("w32", r32)], scale=1.0 / 128, bias=EPS)
                P.op("dve", lambda e, r32=r32: e.reciprocal(w32[r32][:], w32[r32][:]), [("w32", r32)], [("w32", r32)])
                P.op("dve", lambda e, r32=r32, src=src, dst=dst, sl=sl, gcol=gcol: e.scalar_tensor_tensor(
                    dst[:, sl], src[:, sl], pr[:, gcol:gcol + 1], w32[r32][:], ALU.mult, ALU.mult),
                    [sk, "pr", ("w32", r32)], [dk])
        for p, d in enumerate((1, 4, 16)):
            nblk = NB // d
            bsz = min(4, nblk)
            vt = vts[p]
            bo = CF_DIL + ((p * 2 + hl) * 2) * 128
            bias_prev = cf[:, bo:bo + 128]
            bias_cur = cf[:, bo + 128:bo + 256]
            for r in range(d):
                for n0 in range(0, nblk, bsz):
                    ncol = bsz * 128

                    def cols(n, cnt=1):
                        start = n * 128 * d + r
                        return slice(start, start + (cnt * 128 - 1) * d + 1, d) if d > 1 else slice(start, start + cnt * 128)
                    has_prev = [(n0 + u) > 0 for u in range(bsz)]
                    bc = next_bank()
                    for u in range(bsz):
                        P.mm(banks[bc][:, u * 128:(u + 1) * 128], kn[:, cols(n0 + u)], qn[:, cols(n0 + u)], True, True,
                             [Bk(0), Bk(1)], [("ps", bc)])
                    bp = next_bank()
                    for u in range(bsz):
                        if has_prev[u]:
                            P.mm(banks[bp][:, u * 128:(u + 1) * 128], kn[:, cols(n0 + u - 1)], qn[:, cols(n0 + u)],
                                 True, True, [Bk(0), Bk(1)], [("ps", bp)])
                    u0 = 0 if has_prev[0] else 1
                    sc = n32()
                    P.op("dve", lambda e, sc=sc, bc=bc, ncol=ncol, bsz=bsz, bias_cur=bias_cur: e.scalar_tensor_tensor(
                        w32[sc][:, 0:ncol].rearrange("p (u t) -> p u t", u=bsz),
                        banks[bc][:, 0:ncol].rearrange("p (u t) -> p u t", u=bsz), SCALE,
                        bias_cur.unsqueeze(1).to_broadcast([128, bsz, 128]), ALU.mult, ALU.add),
                        [("ps", bc), "cf"], [("w32", sc)])
                    pc = n16() % 4
                    P.act(w16[pc][:, 0:ncol], w32[sc][:, 0:ncol], AF.Exp, [("w32", sc)], [("w16", pc)])
                    pp = None
                    if u0 < bsz:
                        sp_ = n32()
                        nu = bsz - u0
                        P.op("dve", lambda e, sp_=sp_, bp=bp, u0=u0, nu=nu, ncol=ncol, bias_prev=bias_prev: e.scalar_tensor_tensor(
                            w32[sp_][:, u0 * 128:ncol].rearrange("p (u t) -> p u t", u=nu),
                            banks[bp][:, u0 * 128:ncol].rearrange("p (u t) -> p u t", u=nu), SCALE,
                            bias_prev.unsqueeze(1).to_broadcast([128, nu, 128]), ALU.mult, ALU.add),
                            [("ps", bp), "cf"], [("w32", sp_)])
                        pp = n16() % 4
                        P.act(w16[pp][:, u0 * 128:ncol], w32[sp_][:, u0 * 128:ncol], AF.Exp, [("w32", sp_)], [("w16", pp)])
                    bn = next_bank()
                    bd = next_bank()
                    for u in range(bsz):
                        us = slice(u * 128, (u + 1) * 128)
                        blk = r * nblk + n0 + u
                        if has_prev[u]:
                            P.mm(banks[bn][:, us], vt[:, blk - 1, :], w16[pp][:, us], True, False,
                                 ["T%d" % (p + 1), ("w16", pp)], [("ps", bn)])
                        P.mm(banks[bn][:, us], vt[:, blk, :], w16[pc][:, us], not has_prev[u], True,
                             ["T%d" % (p + 1), ("w16", pc)], [("ps", bn)])
                    for u in range(bsz):
                        us = slice(u * 128, (u + 1) * 128)
                        if has_prev[u]:
                            P.mm(banks[bd][:, us], ones_b, w16[pp][:, us], True, False, ["cb", ("w16", pp)], [("ps", bd)])
                        P.mm(banks[bd][:, us], ones_b, w16[pc][:, us], not has_prev[u], True,
                             ["cb", ("w16", pc)], [("ps", bd)])
                    cs_ = cols(n0, bsz)
                    if p == 0:
                        P.act(Nacc[:, cs_], banks[bn][:, 0:ncol], AF.Copy, [("ps", bn)], [F(2)])
                        P.op("dve", lambda e, bd=bd, cs_=cs_, ncol=ncol: e.tensor_copy(Dacc[:, cs_], banks[bd][:, 0:ncol]),
                             [("ps", bd)], [F(3)])
                    else:
                        P.op("dve", lambda e, bn=bn, cs_=cs_, ncol=ncol: e.tensor_tensor(
                            Nacc[:, cs_], banks[bn][:, 0:ncol], Nacc[:, cs_], ALU.add), [("ps", bn), F(2)], [F(2)])
                        P.op("dve", lambda e, bd=bd, cs_=cs_, ncol=ncol: e.tensor_tensor(
                            Dacc[:, cs_], banks[bd][:, 0:ncol], Dacc[:, cs_], ALU.add), [("ps", bd), F(3)], [F(3)])
        P.op("dve", lambda e: e.reciprocal(Dacc[:], Dacc[:]), [F(3)], [F(3)])
        P.op("dve", lambda e: e.tensor_tensor(Nacc[:], Nacc[:], Dacc[:], ALU.mult), [F(2), F(3)], [F(2)])
        store_y(2 * 2 + hl, Nacc[:], [F(2)], "F2")

    def mlstm(hl):
        q32, k32, o32, H = Fb[0], Fb[1], Fb[2], Fb[3]
        Qs, Kb, SD = Bb[1], Bb[2], Bb[3]
        Va, Kt, Kw = T0, T2, T1
        P.dma("sp", k32[:], pf[:, MK * 2 + hl, :], [], [F(1)], F(1))
        P.dma("sp", o32[:], pf[:, MO * 2 + hl, :], [], [F(2)], F(2))
        ck = (0 * 2 + hl) * 128
        cv = (1 * 2 + hl) * 128
        load_tm(Kt, 128, ck, "T2")
        load_tm(Va, 128, cv, "T0")
        P.op("pool", lambda e: e.memset(Va[:, :, 128:256], 1.0), [], ["T0"])
        icol = smalls[:, 64:96]
        fcol = smalls[:, 96:128]
        wit = smalls[:, 128:160]
        ebc = smalls[:, 160:192]
        gcol = smalls[:, 192:224]
        P.dma("sp", G32[:], pg.rearrange("g (c p) -> c g p", p=128), [], ["G32"], "G32")
        for (dst, g, key) in ((icol, hl, "icol"), (fcol, 2 + hl, "fcol")):
            bt = next_bank()
            P.mm(banks[bt][:, 0:32], G32[:, g, :], cf[0:32, CF_ID:CF_ID + 32], True, True, ["G32", "cf"], [("ps", bt)])
            P.op("dve", lambda e, bt=bt, dst=dst: e.tensor_copy(dst, banks[bt][:, 0:32]), [("ps", bt)], [key])
        P.op("pool", lambda e: e.tensor_copy(Kb[:], k32[:]), [F(1)], [Bk(2)])
        idf = cf[:, CF_ID:CF_ID + 128]
        onesf = cf[:, CF_ONE:CF_ONE + 128]
        negf = cf[:, CF_NEG1:CF_NEG1 + 128]
        ltf = cf[:, CF_LT:CF_LT + 128]
        maskf = cf[:, CF_MM:CF_MM + 128]
        P.op("dve", lambda e: e.tensor_scalar(fcol, fcol, pr[:, 22 + hl:23 + hl], None, ALU.add), ["fcol", "pr"], ["fcol"])
        P.act(fcol, fcol, AF.Exp, ["fcol"], ["fcol"], scale=-1.0)
        P.act(fcol, fcol, AF.Ln, ["fcol"], ["fcol"], bias=1.0)
        b = next_bank()
        P.mm(banks[b][:, 0:32], ltf, fcol, True, True, ["cf", "fcol"], [("ps", b)])
        P.op("dve", lambda e, b=b: e.tensor_copy(wit, banks[b][:, 0:32]), [("ps", b)], ["wit"])
        P.act(ebc, wit, AF.Exp, ["wit"], ["ebc"], scale=-1.0)
        P.op("dve", lambda e: e.scalar_tensor_tensor(gcol, icol, pr[:, 20 + hl:21 + hl], wit, ALU.add, ALU.add),
             ["icol", "pr", "wit"], ["gcol"])
        eblast = smalls[:, 0:NB]
        wcols = smalls[:, 32:64]
        bsel = next_bank()
        P.mm(banks[bsel][:, 0:32], cf[:, CF_SEL:CF_SEL + 128], ebc, True, True, ["cf", "ebc"], [("ps", bsel)])
        P.op("dve", lambda e: e.tensor_copy(eblast, banks[bsel][:, 0:32]), [("ps", bsel)], ["eblast"])
        if stage < 2:
            return
        for c0 in range(0, NB, 4):
            sl = slice(c0 * 128, (c0 + 4) * 128)
            d1 = n32()
            P.op("dve", lambda e, d1=d1, c0=c0: e.tensor_tensor(
                w32[d1][:].rearrange("p (u t) -> p u t", u=4), idf.unsqueeze(1).to_broadcast([128, 4, 128]),
                ebc[:, c0:c0 + 4].unsqueeze(2).to_broadcast([128, 4, 128]), ALU.mult), ["cf", "ebc"], [("w32", d1)])
            if stage < 2.1:
                continue
            be = next_bank()
            P.mm(banks[be][:], onesf, w32[d1][:], True, True, ["cf", ("w32", d1)], [("ps", be)])
            if stage < 2.2:
                continue
            P.op("dve", lambda e, be=be, sl=sl: e.tensor_tensor(Qs[:, sl], q32[:, sl], banks[be][:], ALU.mult),
                 [("ps", be), F(0)], [Bk(1)])
            if stage < 2.4:
                continue
            bs_ = next_bank()
            for u in range(4):
                c = c0 + u
                P.mm(banks[bs_][:, u * 128:(u + 1) * 128], Kb[:, c * 128:(c + 1) * 128], q32b(c), True, True,
                     [Bk(2), Bk(0)], [("ps", bs_)])
            d2 = n32()
            P.op("dve", lambda e, d2=d2, c0=c0: e.tensor_tensor(
                w32[d2][:].rearrange("p (u t) -> p u t", u=4), idf.unsqueeze(1).to_broadcast([128, 4, 128]),
                wit[:, c0:c0 + 4].unsqueeze(2).to_broadcast([128, 4, 128]), ALU.mult), ["cf", "wit"], [("w32", d2)])
            ba = next_bank()
            P.mm(banks[ba][:], negf, w32[d2][:], True, True, ["cf", ("w32", d2)], [("ps", ba)])
            a32 = n32()
            P.op("dve", lambda e, a32=a32, ba=ba, c0=c0: e.tensor_tensor(
                w32[a32][:].rearrange("p (u t) -> p u t", u=4), banks[ba][:].rearrange("p (u t) -> p u t", u=4),
                gcol[:, c0:c0 + 4].unsqueeze(2).to_broadcast([128, 4, 128]), ALU.add),
                [("ps", ba), "gcol"], [("w32", a32)])
            P.op("dve", lambda e, a32=a32: e.tensor_tensor(
                w32[a32][:].rearrange("p (u t) -> p u t", u=4), w32[a32][:].rearrange("p (u t) -> p u t", u=4),
                maskf.unsqueeze(1).to_broadcast([128, 4, 128]), ALU.add), [("w32", a32), "cf"], [("w32", a32)])
            if stage < 2.5:
                continue
            P.act(w32[a32][:], w32[a32][:], AF.Exp, [("w32", a32)], [("w32", a32)])
            P.op("dve", lambda e, a32=a32, bs_=bs_, sl=sl: e.tensor_tensor(SD[:, sl], banks[bs_][:], w32[a32][:], ALU.mult),
                 [("ps", bs_), ("w32", a32)], [Bk(3)])
            P.op("pool", lambda e, a32=a32, c0=c0: e.tensor_copy(wcols[:, c0:c0 + 4], w32[a32][:, 127:512:128]),
                 [("w32", a32)], ["wcols"])
        if stage < 3:
            return
        P.op("dve", lambda e: e.tensor_tensor(Kw[:], Kt[:], wcols.unsqueeze(2).to_broadcast([128, NB, 128]), ALU.mult),
             ["T2", "wcols"], ["T1"])
        for c in range(NB - 1):
            b = next_bank()
            P.mm(banks[b][:, 0:256], Kw[:, c, :], Va[:, c, :], True, True, ["T1", "T0"], [("ps", b)])
            cur = Sf[c % 2]
            prv = Sf[(c + 1) % 2]
            if c == 0:
                P.op("dve", lambda e, b=b, cur=cur: e.tensor_copy(cur[:], banks[b][:, 0:256]), [("ps", b)], [("Sf", c % 2)])
            else:
                P.op("dve", lambda e, b=b, cur=cur, prv=prv, c=c: e.scalar_tensor_tensor(
                    cur[:], prv[:], eblast[:, c:c + 1], banks[b][:, 0:256], ALU.mult, ALU.add),
                    [("ps", b), ("Sf", (c + 1) % 2), "eblast"], [("Sf", c % 2)])
            P.act(Sbf[:, c, :], cur[:], AF.Copy, [("Sf", c % 2)], [("Sbf", c), F(4)])
        if stage < 4:
            return
        for c0 in range(0, NB, 4):
            sl = slice(c0 * 128, (c0 + 4) * 128)
            bn = next_bank()
            bd = next_bank()
            for u in range(4):
                c = c0 + u
                us = slice(u * 128, (u + 1) * 128)
                tsl = slice(c * 128, (c + 1) * 128)
                P.mm(banks[bn][:, us], Va[:, c, 0:128], SD[:, tsl], True, c == 0, ["T0", Bk(3)], [("ps", bn)])
                if c > 0:
                    P.mm(banks[bn][:, us], Sbf[:, c - 1, 0:128], Qs[:, tsl], False, True,
                         [("Sbf", c - 1), F(4), Bk(1)], [("ps", bn)])
            for u in range(4):
                c = c0 + u
                us = slice(u * 128, (u + 1) * 128)
                tsl = slice(c * 128, (c + 1) * 128)
                P.mm(banks[bd][:, us], ones_b, SD[:, tsl], True, c == 0, ["cb", Bk(3)], [("ps", bd)])
                if c > 0:
                    P.mm(banks[bd][:, us], Sbf[:, c - 1, 128:256], Qs[:, tsl], False, True,
                         [("Sbf", c - 1), F(4), Bk(1)], [("ps", bd)])
            d32 = n32()
            P.act(w32[d32][:], banks[bd][:], AF.Abs, [("ps", bd)], [("w32", d32)])
            P.op("dve", lambda e, d32=d32: e.tensor_scalar(w32[d32][:], w32[d32][:], 128 ** 0.5, None, ALU.max),
                 [("w32", d32)], [("w32", d32)])
            P.op("dve", lambda e, d32=d32: e.reciprocal(w32[d32][:], w32[d32][:]), [("w32", d32)], [("w32", d32)])
            P.op("dve", lambda e, d32=d32, bn=bn, sl=sl: e.tensor_tensor(H[:, sl], banks[bn][:], w32[d32][:], ALU.mult),
                 [("ps", bn), ("w32", d32)], [F(3)])
            sq = n16() % 4
            P.act(w16[sq][:], H[:, sl], AF.Square, [F(3)], [("w16", sq)])
            b = next_bank()
            P.mm(banks[b][:], ones_b, w16[sq][:], True, True, ["cb", ("w16", sq)], [("ps", b)])
            r32 = n32()
            P.act(w32[r32][:], banks[b][:], AF.Sqrt, [("ps", b)], [("w32", r32)], scale=1.0 / 128, bias=EPS)
            P.op("dve", lambda e, r32=r32: e.reciprocal(w32[r32][:], w32[r32][:]), [("w32", r32)], [("w32", r32)])
            P.op("dve", lambda e, r32=r32, sl=sl: e.scalar_tensor_tensor(
                H[:, sl], H[:, sl], pr[:, 18 + hl:19 + hl], w32[r32][:], ALU.mult, ALU.mult),
                [F(3), "pr", ("w32", r32)], [F(3)])
            P.act(o32[:, sl], o32[:, sl], AF.Sigmoid, [F(2)], [F(2)])
            P.op("dve", lambda e, sl=sl: e.tensor_tensor(H[:, sl], H[:, sl], o32[:, sl], ALU.mult), [F(3), F(2)], [F(3)])
        store_y(1 * 2 + hl, H[:], [F(3)], "F3")

    Qb = Bb[0]

    def q32b(c):
        return Qb[:, c * 128:(c + 1) * 128]

    def mlstm_pre(hl):
        pass

    for j in range(2):
        if "lru" in do:
            lru(j)
        if "mlstm" in do:
            P.dma("sp", Fb[0][:], pf[:, MQ * 2 + j, :], [], [F(0)], F(0))
            P.op("pool", lambda e: e.tensor_copy(Qb[:], Fb[0][:]), [F(0)], [Bk(0)])
            mlstm(j)
        if "dil" in do:
            dil(j)
        if "sb" in do:
            sb(j)
    P.emit()
    return nc, P
def build_W(nt):
    nc = bass.Bass("TRN2", target_bir_lowering=False)
    wf = nc.dram_tensor("wf", [nt, 128, 2048], F32, kind="ExternalInput").ap()
    wb = nc.dram_tensor("wb", [nt, 128, 2048], BF16, kind="ExternalOutput").ap()
    NS = 4
    s32 = nc.alloc_sbuf_tensor("s32", [128, NS, 2048], F32)
    s16 = nc.alloc_sbuf_tensor("s16", [128, NS, 2048], BF16)
    P = Prog(nc)
    for i in range(nt):
        s = i % NS
        P.dma("sp", s32[:, s, :], wf[i], [], [("a", s)], ("a", s))
        if i % 3 == 0:
            P.op("dve", lambda e, s=s: e.tensor_copy(s16[:, s, :], s32[:, s, :]), [("a", s)], [("b", s)])
        elif i % 3 == 1:
            P.op("pool", lambda e, s=s: e.tensor_copy(s16[:, s, :], s32[:, s, :]), [("a", s)], [("b", s)])
        else:
            P.act(s16[:, s, :], s32[:, s, :], AF.Copy, [("a", s)], [("b", s)])
        P.dma("sp", wb[i], s16[:, s, :], [("b", s)], [("o", i)], ("b", s))
    P.emit()
    return nc


def make_prm(ins, l, hp):
    prm = np.zeros((128, NPRM), np.float32)
    for j in range(2):
        blk = hp * 2 + j
        sl = slice(blk * 128, (blk + 1) * 128)
        prm[:, j * 8 + 0:j * 8 + 4] = ins["lru_conv_w"][l][:, sl].T
        prm[:, j * 8 + 4] = ins["lru_conv_b"][l][sl]
        prm[:, j * 8 + 5] = ins["lru_b_a"][l][sl]
        prm[:, j * 8 + 6] = ins["lru_b_x"][l][sl]
        prm[:, j * 8 + 7] = ins["lru_lambda"][l][sl]
        prm[:, 18 + j] = ins["group_out_gain"][l][512 + blk * 128:512 + (blk + 1) * 128]
        prm[:, 20 + j] = ins["mlstm_ig_bias"][l][blk]
        prm[:, 22 + j] = ins["mlstm_fg_bias"][l][blk]
    prm[:, 16] = ins["attn_q_gain"][l]
    prm[:, 17] = ins["attn_k_gain"][l]
    wax = np.zeros((128, 4, 128), np.float32)
    for j in range(2):
        wax[:, j * 2, :] = ins["lru_w_a"][l][hp * 2 + j]
        wax[:, j * 2 + 1, :] = ins["lru_w_x"][l][hp * 2 + j]
    return prm, wax


_CACHE = {}


def _prog(key, fn):
    if key not in _CACHE:
        _CACHE[key] = fn()
    return _CACHE[key]


CORES = list(range(8))


def kernel(**ins):
    ins = {k: np.asarray(v) for k, v in ins.items()}
    x = ins["x"]
    tiles = []
    seg = {}
    for l in range(2):
        a = tiles_ffn(ins["ffn1_w_gate"][l], ins["ffn1_w_up"][l], ins["ffn1_w_down"][l]) + tiles_win(ins["w_in"][l])
        c = tiles_wout(ins["w_out"][l]) + tiles_ffn(ins["ffn2_w_gate"][l], ins["ffn2_w_up"][l], ins["ffn2_w_down"][l])
        seg[("A", l)] = (len(tiles), len(tiles) + len(a)); tiles += a
        seg[("C", l)] = (len(tiles), len(tiles) + len(c)); tiles += c
    nt = len(tiles)
    per = -(-nt // 8)
    wf = np.zeros((per * 8, 128, 2048), np.float32)
    wf[:nt] = np.stack(tiles)
    del tiles
    ncW = _prog(("W", per), lambda: build_W(per))
    res = run_bass_kernel_spmd(ncW, [{"wf": wf[c * per:(c + 1) * per]} for c in CORES], core_ids=CORES)
    wb = np.concatenate([np.asarray(r["wb"]) for r in res.results], axis=0)
    del wf

    def wslice(*keys):
        return np.ascontiguousarray(np.concatenate([wb[seg[k][0]:seg[k][1]] for k in keys], axis=0))

    def run_T(has_wout, n_ffn, has_win, xTs, wt, gains, Ys=None):
        nc, ntiles, _ = _prog(("T", has_wout, n_ffn, has_win), lambda: build_T(has_wout, n_ffn, has_win))
        assert wt.shape[0] == ntiles
        maps = []
        for c in CORES:
            m = {"xT": xTs[c], "wt": wt, "gains": gains}
            if has_wout:
                m["Y"] = Ys[c]
            maps.append(m)
        return run_bass_kernel_spmd(nc, maps, core_ids=CORES).results

    def run_B(l, tres):
        nc, _ = _prog(("B",), lambda: build_B())
        maps = []
        for c in CORES:
            b, hp = c // 2, c % 2
            pf_b = np.concatenate([np.asarray(tres[2 * b]["pf"]), np.asarray(tres[2 * b + 1]["pf"])], axis=2)
            pg_b = np.concatenate([np.asarray(tres[2 * b]["pg"]), np.asarray(tres[2 * b + 1]["pg"])], axis=1)
            pt_b = np.concatenate([np.asarray(tres[2 * b]["pt"]), np.asarray(tres[2 * b + 1]["pt"])], axis=0)
            idx = [ni * 4 + hp * 2 + hl for ni in range(9) for hl in range(2)]
            pf = np.ascontiguousarray(pf_b[:, idx, :])
            pg = np.ascontiguousarray(pg_b[[hp * 2, hp * 2 + 1, 4 + hp * 2, 4 + hp * 2 + 1], :])
            cols = np.concatenate([np.arange(g * 512 + (hp * 2 + hl) * 128, g * 512 + (hp * 2 + hl + 1) * 128)
                                   for g in range(4) for hl in range(2)])
            pt = np.ascontiguousarray(pt_b[:, cols])
            prm, wax = make_prm(ins, l, hp)
            cb, cf = make_consts(hp)
            maps.append(dict(pf=pf, pg=pg, pt=pt, prm=prm, wax=wax, cb=cb, cf=cf))
        return run_bass_kernel_spmd(nc, maps, core_ids=CORES).results

    def y_for_T(bres):
        Ys = []
        for c in CORES:
            b, j = c // 2, c % 2
            Yt = np.zeros((128, 16, 2048), np.float32)
            for hp in range(2):
                Yb = np.asarray(bres[2 * b + hp]["Y"])
                for m in range(4):
                    for hl in range(2):
                        Yt[:, m * 4 + hp * 2 + hl, :] = Yb[:, m * 2 + hl, j * 2048:(j + 1) * 2048]
            Ys.append(Yt)
        return Ys

    def gcat(*gs):
        return np.ascontiguousarray(np.concatenate([gain_fm(g) for g in gs], axis=1))

    xTs = []
    for c in CORES:
        b, j = c // 2, c % 2
        xTs.append(np.ascontiguousarray(x[b, j * 2048:(j + 1) * 2048, :].T.reshape(KC, 128, 2048).transpose(1, 0, 2)))
    t0 = run_T(False, 1, True, xTs, wslice(("A", 0)), gcat(ins["ffn1_norm"][0], ins["mix_norm"][0]))
    b0 = run_B(0, t0)
    t1 = run_T(True, 2, True, [np.asarray(r["oT"]) for r in t0], wslice(("C", 0), ("A", 1)),
               gcat(ins["group_out_gain"][0], ins["ffn2_norm"][0], ins["ffn1_norm"][1], ins["mix_norm"][1]), y_for_T(b0))
    del t0, b0
    b1 = run_B(1, t1)
    t2 = run_T(True, 1, False, [np.asarray(r["oT"]) for r in t1], wslice(("C", 1)),
               gcat(ins["group_out_gain"][1], ins["ffn2_norm"][1]), y_for_T(b1))
    out = np.zeros((4, 4096, 2048), np.float32)
    for c in CORES:
        b, j = c // 2, c % 2
        oT = np.asarray(t2[c]["oT"])
        out[b, j * 2048:(j + 1) * 2048, :] = oT.transpose(1, 0, 2).reshape(2048, 2048).T
    return out
```
